# Optimizing a Trainium2 kernel written in Bass

```python
import jax, jax.numpy as jnp
from jax import lax
import numpy as np

D_MODEL = 2048
BATCH = 4
SEQ = 8192
DEPTH = 1

GRID_W = 64
CTX_LEN = 256
ROPE_BASE = 10000.0
EPS = 1e-6
Q_BLK = 128

A_HEADS = 16
A_KV_HEADS = 4
A_HEAD_DIM = 64
WINDOW = 128

B_HEADS = 8
B_NOPE_DIM = 128
B_ROPE_DIM = 64
B_QK_DIM = B_NOPE_DIM + B_ROPE_DIM
B_V_DIM = 128
Q_LORA_RANK = 512
KV_LORA_RANK = 256

MIX_WIDTH = A_HEADS * A_HEAD_DIM + B_HEADS * B_V_DIM
IN_SPLITS = (A_HEADS * A_HEAD_DIM, A_KV_HEADS * A_HEAD_DIM, A_KV_HEADS * A_HEAD_DIM,
             Q_LORA_RANK, KV_LORA_RANK, B_ROPE_DIM)
IN_COLS = sum(IN_SPLITS)

N_EXPERTS = 64
TOP_K = 8
N_GROUPS = 8
TOPK_GROUPS = 4
EXPERT_FF = 512
SHARED_FF = 512
ROUTED_SCALE = 2.5
EXPERT_BLK = 128

kernel_name = "hybrid_swa_mla_moe_dit_block"


def rmsnorm(x, g):
    xf = x.astype(jnp.float32)
    y = xf * lax.rsqrt(jnp.mean(xf * xf, axis=-1, keepdims=True) + EPS)
    return (y * g.astype(jnp.float32)).astype(x.dtype)


def modulate(x, shift, scale):
    return x * (1 + scale) + shift


def heads(t, n):
    return t.reshape(*t.shape[:-1], n, t.shape[-1] // n)


def axial_rope_tables(rows, rot_dim):
    r = jnp.repeat(jnp.arange(rows, dtype=jnp.float32), GRID_W)
    cidx = jnp.tile(jnp.arange(GRID_W, dtype=jnp.float32), rows)
    n_freq = rot_dim // 4
    inv = ROPE_BASE ** (-jnp.arange(n_freq, dtype=jnp.float32) / n_freq)
    ang = jnp.concatenate([r[:, None] * inv, cidx[:, None] * inv], axis=-1)
    return jnp.cos(ang), jnp.sin(ang)


def rope_or_id(x, rope):
    if rope is None:
        return x
    cos, sin = rope
    c = cos[None, :, None, :].astype(x.dtype)
    s = sin[None, :, None, :].astype(x.dtype)
    x1, x2 = jnp.split(x, 2, axis=-1)
    return jnp.concatenate([x1 * c - x2 * s, x2 * c + x1 * s], axis=-1)


def softmax_with_sink(scores, sink):
    if sink is None:
        return jax.nn.softmax(scores, axis=-1)
    full = jnp.concatenate([scores, jnp.broadcast_to(sink, scores.shape[:-1] + (1,))], axis=-1)
    return jax.nn.softmax(full, axis=-1)[..., :-1]


def dense_attn(q, k, v, sink):
    b, lq, h, d = q.shape
    hkv = k.shape[2]
    g = h // hkv
    qg = q.reshape(b, lq, hkv, g, d)
    s = jnp.einsum('bqhgd,bkhd->bhgqk', qg, k).astype(jnp.float32) * (d ** -0.5)
    sink_g = None if sink is None else sink.reshape(hkv, g)[None, :, :, None, None].astype(jnp.float32)
    p = softmax_with_sink(s, sink_g).astype(v.dtype)
    return jnp.einsum('bhgqk,bkhd->bqhgd', p, v).reshape(b, lq, h * v.shape[-1])


def windowed_gqa(q, k, v, k_ctx, v_ctx, sink):
    b, s_len, h, d = q.shape
    hkv = k.shape[2]
    g = h // hkv
    nb = s_len // Q_BLK
    span = Q_BLK + 2 * WINDOW
    pad = ((0, 0), (WINDOW, WINDOW), (0, 0), (0, 0))
    kp = jnp.pad(k, pad)
    vp = jnp.pad(v, pad)
    q_blocks = q.reshape(b, nb, Q_BLK, hkv, g, d).transpose(1, 0, 2, 3, 4, 5)
    r = jnp.arange(Q_BLK)
    j = jnp.arange(span)
    in_band = jnp.abs(j[None, :] - WINDOW - r[:, None]) <= WINDOW
    sink_g = sink.reshape(hkv, g)[None, :, :, None, None].astype(jnp.float32)
    scale = d ** -0.5

    def block(args):
        qb, i = args
        start = i * Q_BLK
        kb = lax.dynamic_slice_in_dim(kp, start, span, axis=1)
        vb = lax.dynamic_slice_in_dim(vp, start, span, axis=1)
        kpos = start - WINDOW + j
        mask = in_band & ((kpos >= 0) & (kpos < s_len))[None, :]
        s_win = jnp.einsum('bqhgd,bkhd->bhgqk', qb, kb).astype(jnp.float32) * scale
        s_win = jnp.where(mask, s_win, -jnp.inf)
        s_ctx = jnp.einsum('bqhgd,bkhd->bhgqk', qb, k_ctx).astype(jnp.float32) * scale
        p = softmax_with_sink(jnp.concatenate([s_win, s_ctx], axis=-1), sink_g).astype(v.dtype)
        return (jnp.einsum('bhgqk,bkhd->bqhgd', p[..., :span], vb)
                + jnp.einsum('bhgqk,bkhd->bqhgd', p[..., span:], v_ctx))

    out = lax.map(block, (q_blocks, jnp.arange(nb)))
    return out.transpose(1, 0, 2, 3, 4, 5).reshape(b, s_len, h * d)


def mla_q(cq, g, w_uq, rope):
    b, l, _ = cq.shape
    q = (rmsnorm(cq, g) @ w_uq).reshape(b, l, B_HEADS, B_QK_DIM)
    q_nope, q_rope = jnp.split(q, [B_NOPE_DIM], axis=-1)
    return jnp.concatenate([q_nope, rope_or_id(q_rope, rope)], axis=-1)


def mla_kv(ckv, kr, g, w_ukv, rope):
    b, l, _ = ckv.shape
    kv = (rmsnorm(ckv, g) @ w_ukv).reshape(b, l, B_HEADS, B_NOPE_DIM + B_V_DIM)
    k_nope, v = jnp.split(kv, [B_NOPE_DIM], axis=-1)
    k_rope = rope_or_id(kr[:, :, None, :], rope)
    k = jnp.concatenate([k_nope, jnp.broadcast_to(k_rope, (b, l, B_HEADS, B_ROPE_DIM))], axis=-1)
    return k, v


def mla_attn(q, k, v, k_ctx, v_ctx):
    b, s_len, h, dq = q.shape
    nb = s_len // Q_BLK
    kk = jnp.concatenate([k, k_ctx], axis=1)
    vv = jnp.concatenate([v, v_ctx], axis=1)
    q_blocks = q.reshape(b, nb, Q_BLK, h, dq).transpose(1, 0, 2, 3, 4)
    out = lax.map(lambda qb: dense_attn(qb, kk, vv, None), q_blocks)
    return out.transpose(1, 0, 2, 3).reshape(b, s_len, h * B_V_DIM)


def split_proj(h, w_in):
    offs = np.cumsum(IN_SPLITS)[:-1].tolist()
    return jnp.split(h @ w_in, offs, axis=-1)


def swiglu(x, wg, wu, wd):
    return (jax.nn.silu(x @ wg) * (x @ wu)) @ wd


def route(hf, w_router, bias):
    t = hf.shape[0]
    s = jax.nn.sigmoid(hf.astype(jnp.float32) @ w_router.astype(jnp.float32))
    sel = s + bias.astype(jnp.float32)
    grp = sel.reshape(t, N_GROUPS, N_EXPERTS // N_GROUPS)
    g_score = lax.top_k(grp, 2)[0].sum(-1)
    _, g_idx = lax.top_k(g_score, TOPK_GROUPS)
    g_mask = jax.nn.one_hot(g_idx, N_GROUPS, dtype=jnp.float32).sum(1) > 0
    sel = jnp.where(jnp.repeat(g_mask, N_EXPERTS // N_GROUPS, axis=1), sel, -jnp.inf)
    _, idx = lax.top_k(sel, TOP_K)
    w = jnp.take_along_axis(s, idx, axis=1)
    w = w / jnp.sum(w, axis=-1, keepdims=True) * ROUTED_SCALE
    return idx, w.astype(hf.dtype)


def routed_experts(hf, idx, wts, w_gate, w_up, w_down):
    t, d = hf.shape
    a = t * TOP_K
    e_flat = idx.reshape(a)
    order = jnp.argsort(e_flat)
    e_sorted = e_flat[order]
    tok_sorted = (order // TOP_K).astype(jnp.int32)
    w_sorted = wts.reshape(a)[order]
    counts = jnp.bincount(e_flat, length=N_EXPERTS)
    padded = (counts + EXPERT_BLK - 1) // EXPERT_BLK * EXPERT_BLK
    pad_end = jnp.cumsum(padded)
    pad_start = pad_end - padded
    start = jnp.cumsum(counts) - counts
    dest = pad_start[e_sorted] + jnp.arange(a) - start[e_sorted]
    n_blk = -(-a // EXPERT_BLK) + N_EXPERTS
    p_len = n_blk * EXPERT_BLK
    tok_buf = jnp.full((p_len,), t, jnp.int32).at[dest].set(tok_sorted)
    w_buf = jnp.zeros((p_len,), hf.dtype).at[dest].set(w_sorted)
    blk_expert = jnp.minimum(
        jnp.searchsorted(pad_end, jnp.arange(n_blk) * EXPERT_BLK, side='right'), N_EXPERTS - 1)
    h_pad = jnp.concatenate([hf, jnp.zeros((1, d), hf.dtype)], axis=0)

    def step(acc, blk):
        tok, w, e = blk
        yb = swiglu(h_pad[tok], w_gate[e], w_up[e], w_down[e])
        return acc.at[tok].add(yb * w[:, None]), None

    acc, _ = lax.scan(step, jnp.zeros((t + 1, d), hf.dtype),
                      (tok_buf.reshape(n_blk, EXPERT_BLK), w_buf.reshape(n_blk, EXPERT_BLK), blk_expert))
    return acc[:t]


def moe_ffn(h, w_router, router_bias, w_gate, w_up, w_down, ws_gate, ws_up, ws_down):
    b, l, d = h.shape
    hf = h.reshape(b * l, d)
    idx, wts = route(hf, w_router, router_bias)
    y = routed_experts(hf, idx, wts, w_gate, w_up, w_down) + swiglu(hf, ws_gate, ws_up, ws_down)
    return y.reshape(b, l, d)


def setup_inputs(seed: int = 0) -> dict:
    key = jax.random.key(seed)
    ks = jax.random.split(key, 32)
    D = D_MODEL

    def nrm(k, shape, scale):
        return jax.random.normal(k, shape, jnp.float32) * scale

    return {
        "x": nrm(ks[0], (BATCH, SEQ, D), 1.0),
        "c": nrm(ks[1], (BATCH, D), 1.0),
        "ctx": nrm(ks[2], (BATCH, CTX_LEN, D), 1.0),
        "c_ctx": nrm(ks[3], (D,), 1.0),
        "w_mod": nrm(ks[4], (DEPTH, D, 6 * D), 0.5 * D ** -0.5),
        "b_mod": nrm(ks[5], (DEPTH, 6 * D), 0.02),
        "norm_attn_g": 1.0 + nrm(ks[6], (DEPTH, D), 0.01),
        "norm_ffn_g": 1.0 + nrm(ks[7], (DEPTH, D), 0.01),
        "w_in": nrm(ks[8], (DEPTH, D, IN_COLS), D ** -0.5),
        "attn_sink": nrm(ks[9], (DEPTH, A_HEADS), 0.5),
        "q_a_norm_g": 1.0 + nrm(ks[10], (DEPTH, Q_LORA_RANK), 0.01),
        "w_uq": nrm(ks[11], (DEPTH, Q_LORA_RANK, B_HEADS * B_QK_DIM), Q_LORA_RANK ** -0.5),
        "kv_a_norm_g": 1.0 + nrm(ks[12], (DEPTH, KV_LORA_RANK), 0.01),
        "w_ukv": nrm(ks[13], (DEPTH, KV_LORA_RANK, B_HEADS * (B_NOPE_DIM + B_V_DIM)), KV_LORA_RANK ** -0.5),
        "w_out": nrm(ks[14], (DEPTH, MIX_WIDTH, D), MIX_WIDTH ** -0.5),
        "w_router": nrm(ks[15], (DEPTH, D, N_EXPERTS), D ** -0.5),
        "router_bias": nrm(ks[16], (DEPTH, N_EXPERTS), 0.01),
        "w_gate": nrm(ks[17], (DEPTH, N_EXPERTS, D, EXPERT_FF), D ** -0.5),
        "w_up": nrm(ks[18], (DEPTH, N_EXPERTS, D, EXPERT_FF), D ** -0.5),
        "w_down": nrm(ks[19], (DEPTH, N_EXPERTS, EXPERT_FF, D), EXPERT_FF ** -0.5),
        "ws_gate": nrm(ks[20], (DEPTH, D, SHARED_FF), D ** -0.5),
        "ws_up": nrm(ks[21], (DEPTH, D, SHARED_FF), D ** -0.5),
        "ws_down": nrm(ks[22], (DEPTH, SHARED_FF, D), SHARED_FF ** -0.5),
        "norm_final_g": 1.0 + nrm(ks[23], (D,), 0.01),
    }


def reference(x, c, ctx, c_ctx, w_mod, b_mod, norm_attn_g, norm_ffn_g, w_in, attn_sink,
              q_a_norm_g, w_uq, kv_a_norm_g, w_ukv, w_out, w_router, router_bias,
              w_gate, w_up, w_down, ws_gate, ws_up, ws_down, norm_final_g):
    b, s_len, d = x.shape
    ROWS = s_len // GRID_W
    rope_a = axial_rope_tables(ROWS, A_HEAD_DIM)
    rope_b = axial_rope_tables(ROWS, B_ROPE_DIM)

    for l in range(DEPTH):
        last = l == DEPTH - 1
        mod = (jax.nn.silu(c) @ w_mod[l] + b_mod[l])[:, None, :]
        mod_c = jax.nn.silu(c_ctx) @ w_mod[l] + b_mod[l]
        sh_a, sc_a, g_a, sh_f, sc_f, g_f = jnp.split(mod, 6, axis=-1)
        sh_ac, sc_ac, g_ac, sh_fc, sc_fc, g_fc = jnp.split(mod_c, 6, axis=-1)

        h = modulate(rmsnorm(x, norm_attn_g[l]), sh_a, sc_a)
        hc = modulate(rmsnorm(ctx, norm_attn_g[l]), sh_ac, sc_ac)
        qa, ka, va, cq, ckv, kr = split_proj(h, w_in[l])
        qa_c, ka_c, va_c, cq_c, ckv_c, kr_c = split_proj(hc, w_in[l])

        qa = rope_or_id(heads(qa, A_HEADS), rope_a)
        ka = rope_or_id(heads(ka, A_KV_HEADS), rope_a)
        va = heads(va, A_KV_HEADS)
        ka_c = heads(ka_c, A_KV_HEADS)
        va_c = heads(va_c, A_KV_HEADS)
        o_a = windowed_gqa(qa, ka, va, ka_c, va_c, attn_sink[l])

        qb = mla_q(cq, q_a_norm_g[l], w_uq[l], rope_b)
        kb, vb = mla_kv(ckv, kr, kv_a_norm_g[l], w_ukv[l], rope_b)
        kb_c, vb_c = mla_kv(ckv_c, kr_c, kv_a_norm_g[l], w_ukv[l], None)
        o_b = mla_attn(qb, kb, vb, kb_c, vb_c)

        x_new = x + g_a * (jnp.concatenate([o_a, o_b], axis=-1) @ w_out[l])

        if not last:
            qa_c = heads(qa_c, A_HEADS)
            qb_c = mla_q(cq_c, q_a_norm_g[l], w_uq[l], None)
            o_c = jnp.concatenate([dense_attn(qa_c, ka_c, va_c, attn_sink[l]),
                                   dense_attn(qb_c, kb_c, vb_c, None)], axis=-1)
            ctx = ctx + g_ac * (o_c @ w_out[l])
            hc2 = modulate(rmsnorm(ctx, norm_ffn_g[l]), sh_fc, sc_fc)
            ctx = ctx + g_fc * moe_ffn(hc2, w_router[l], router_bias[l], w_gate[l], w_up[l], w_down[l],
                                       ws_gate[l], ws_up[l], ws_down[l])

        x = x_new
        h2 = modulate(rmsnorm(x, norm_ffn_g[l]), sh_f, sc_f)
        x = x + g_f * moe_ffn(h2, w_router[l], router_bias[l], w_gate[l], w_up[l], w_down[l],
                              ws_gate[l], ws_up[l], ws_down[l])

    return rmsnorm(x, norm_final_g)
```

```python
import numpy as np
from contextlib import ExitStack
import ml_dtypes
import concourse.bass as bass
import concourse.mybir as mybir
from concourse.bass_utils import run_bass_kernel_spmd

F32 = mybir.dt.float32
BF16 = mybir.dt.bfloat16
AF = mybir.ActivationFunctionType
ALU = mybir.AluOpType
AX = mybir.AxisListType

D = 2048
SEQ = 8192
OWN = 4096
CTX = 256
NE = 64
EPS = 1e-6
ENGS = ("pe", "act", "dve", "pool", "sp")


class Buf:
    __slots__ = ("name", "writers", "readers", "prev", "sem_stream")

    def __init__(self, name):
        self.name = name
        self.writers = []
        self.readers = []
        self.prev = []
        self.sem_stream = None


class Op:
    __slots__ = ("eng", "fn", "kind", "stream", "idx", "deps", "signal", "sig_val", "waits")

    def __init__(self, eng, fn, kind):
        self.eng = eng
        self.fn = fn
        self.kind = kind
        self.stream = None
        self.idx = 0
        self.deps = []
        self.signal = False
        self.sig_val = 0
        self.waits = []


class Sched:
    def __init__(self, nc):
        self.nc = nc
        self.ops = {e: [] for e in ENGS}
        self.streams = {e: [] for e in ENGS}
        self.n_dma_streams = 0

    def _dma_stream(self, buf):
        if buf.sem_stream is None:
            buf.sem_stream = "dma%d" % self.n_dma_streams
            self.n_dma_streams += 1
            self.streams[buf.sem_stream] = []
        return buf.sem_stream

    def op(self, eng, fn, reads=(), writes=(), partial=(), kind="c", sem_buf=None):
        o = Op(eng, fn, kind)
        if kind == "dma":
            sb = sem_buf
            if sb is None:
                sb = (list(writes) + list(partial) + list(reads))[0]
            o.stream = self._dma_stream(sb)
        else:
            o.stream = eng
        st = self.streams[o.stream]
        o.idx = len(st)
        st.append(o)
        deps = []
        for b in reads:
            deps.extend(b.writers)
            b.readers.append(o)
        for b in writes:
            b.prev = b.readers + b.writers
            b.readers = []
            b.writers = [o]
            deps.extend(b.prev)
        for b in partial:
            if b.readers:
                b.prev = b.readers + b.writers
                b.readers = []
                b.writers = []
            deps.extend(b.prev)
            b.writers.append(o)
        o.deps = [d for d in deps if d is not o]
        self.ops[eng].append(o)
        return o

    def barrier(self):
        lasts = [st[-1] for st in self.streams.values() if st]
        for e in ENGS:
            o = Op(e, None, "bar")
            o.deps = list(lasts)
            self.ops[e].append(o)

    def resolve(self):
        for e in ENGS:
            waited = {}
            for o in self.ops[e]:
                best = {}
                for d in o.deps:
                    if d.stream == "pe" and e == "pe" and o.kind != "bar":
                        continue
                    cur = best.get(d.stream)
                    if cur is None or d.idx > cur.idx:
                        best[d.stream] = d
                for s, d in best.items():
                    if waited.get(s, -1) >= d.idx:
                        continue
                    waited[s] = d.idx
                    d.signal = True
                    o.waits.append(d)
        for s, st in self.streams.items():
            c = 0
            for o in st:
                if o.kind == "dma":
                    c += 16
                    o.sig_val = c
                    o.signal = True
                elif o.signal:
                    c += 1
                    o.sig_val = c

    def emit(self, block, sems):
        def run(e, eng):
            for o in self.ops[e]:
                for d in o.waits:
                    eng.wait_ge(sems[d.stream], d.sig_val)
                if o.fn is None:
                    continue
                ins = o.fn(eng)
                if o.signal:
                    ins.then_inc(sems[o.stream], 16 if o.kind == "dma" else 1)

        @block.sync
        def _(eng):
            run("sp", eng)

        @block.scalar
        def _(eng):
            run("act", eng)

        @block.vector
        def _(eng):
            run("dve", eng)

        @block.gpsimd
        def _(eng):
            run("pool", eng)

        @block.tensor
        def _(eng):
            run("pe", eng)


class Carve:
    def __init__(self, big, nbytes):
        self.big = big
        self.cap = nbytes
        self.top = 0
        self.peak = 0

    def mark(self):
        return self.top

    def release(self, m):
        self.top = m

    def alloc(self, shape, dt):
        nb = 4 if dt == F32 else 2
        n = int(np.prod(shape))
        nbytes = (n * nb + 63) // 64 * 64
        off = self.top
        self.top += nbytes
        self.peak = max(self.peak, self.top)
        assert self.top <= self.cap, "SBUF overflow %d > %d" % (self.top, self.cap)
        self.last_off = off
        return self.view(off, shape, dt)

    def view(self, off, shape, dt):
        nb = 4 if dt == F32 else 2
        n = int(np.prod(shape))
        v = self.big[:, off // 2: off // 2 + n * nb // 2]
        if dt == F32:
            v = v.bitcast(F32)
        if len(shape) == 2:
            v = v.rearrange("p (a b) -> p a b", a=shape[0])
        elif len(shape) == 3:
            v = v.rearrange("p (a b c) -> p a b c", a=shape[0], b=shape[1])
        return v


SBUF_BYTES = 207 * 1024


def build(stage=99, dbg=False, bis=99):
    nc = bass.Bass("TRN2", target_bir_lowering=False)
    S = Sched(nc)

    def din(name, shape, dt=F32):
        return nc.dram_tensor(name, list(shape), dt, kind="ExternalInput").ap()

    def dscr(name, shape, dt):
        return nc.dram_tensor(name, list(shape), dt, kind="Internal").ap()

    xs = din("xs", [SEQ, D])
    ctxb = din("ctxb", [CTX, D])
    cT_d = din("cT", [128, 32])
    w_mod = din("w_mod", [D, 6 * D])
    b_mod2 = din("b_mod2", [2, 6 * D])
    gcol_d = din("gcol", [128, 32])
    w_in = din("w_in", [D, 2368])
    sink_d = din("sink", [128, 16])
    gq_d = din("gq", [128, 512])
    gkv_d = din("gkv", [128, 256])
    w_uq = din("w_uq", [512, 1536])
    w_ukv = din("w_ukv", [256, 2048])
    w_out = din("w_out", [D, D])
    w_router = din("w_router", [D, NE])
    rbias_d = din("rbias", [128, NE])
    NEI = NE if stage >= 4 else 1
    w_gate = din("w_gate", [NEI, D, 512])
    w_up = din("w_up", [NEI, D, 512])
    w_down = din("w_down", [NEI, 512, D])
    ws_gate = din("ws_gate", [D, 512])
    ws_up = din("ws_up", [D, 512])
    ws_down = din("ws_down", [512, D])
    gfin_d = din("gfin", [128, D])
    ropeC_d = din("ropeC", [64, SEQ])
    ropeS_d = din("ropeS", [64, SEQ])
    masks_d = din("masks", [8, 128, 512], BF16)
    identb_d = din("identb", [128, 128], BF16)
    identf_d = din("identf", [128, 128])
    perm_d = din("perm64", [64, 64])
    out_d = nc.dram_tensor("out", [OWN, D], F32, kind="ExternalOutput").ap()

    OTa_d = dscr("OTa", [16, 64, OWN], BF16)
    OTb_d = dscr("OTb", [8, 128, OWN], BF16)
    wexp_d = [dscr("wexp%d" % i, [65, 128, 8192], BF16) for i in range(3)]
    woutA_d = dscr("woutA", [4, 64, 8192], BF16)
    woutB_d = dscr("woutB", [4, 128, 4096], BF16)

    dbg_outs = {}

    def dbg_out(name, shape, dt=F32):
        t = nc.dram_tensor(name, list(shape), dt, kind="ExternalOutput").ap()
        dbg_outs[name] = t
        return t

    es = ExitStack()
    big = es.enter_context(nc.sbuf_tensor("big", [128, SBUF_BYTES // 2], BF16))
    cv = Carve(big, SBUF_BYTES)
    ps = [es.enter_context(nc.psum_tensor("ps%d" % i, [128, 512], F32)) for i in range(8)]
    PB = [Buf("ps%d" % i) for i in range(8)]

    def psb(i):
        return ps[i][:].bitcast(BF16)

    def dma(out, in_, reads=(), writes=(), partial=(), eng="sp", sem_buf=None):
        return S.op(eng, lambda e: e.dma_start(out=out, in_=in_), reads=reads, writes=writes,
                    partial=partial, kind="dma", sem_buf=sem_buf)

    def mm(out, lhsT, rhs, start, stop, reads=(), writes=(), partial=()):
        return S.op("pe", lambda e: e.matmul(out, lhsT=lhsT, rhs=rhs, start=start, stop=stop),
                    reads=reads, writes=writes, partial=partial)

    def tr(out, in_, ident, reads=(), partial=()):
        return S.op("pe", lambda e: e.transpose(out=out, in_=in_, identity=ident), reads=reads, partial=partial)

    def act(out, in_, func, reads=(), writes=(), partial=(), **kw):
        return S.op("act", lambda e: e.activation(out=out, in_=in_, func=func, **kw),
                    reads=reads, writes=writes, partial=partial)

    def vop(eng, name, reads=(), writes=(), partial=(), **kw):
        return S.op(eng, lambda e: getattr(e, name)(**kw), reads=reads, writes=writes, partial=partial)

    identb = cv.alloc([128], BF16)
    identf = cv.alloc([128], F32)
    perm64 = cv.alloc([64], F32)
    onesb = cv.alloc([128], BF16)
    onesf = cv.alloc([128], F32)
    epsb = cv.alloc([1], F32)
    cols = cv.alloc([6, 16], F32)
    ga_bc = cv.alloc([D], F32)
    gf_bc = cv.alloc([D], F32)
    esink = cv.alloc([16], F32)
    B_const = Buf("const")
    B_cols = Buf("cols")
    B_ga = Buf("ga_bc")
    B_gf = Buf("gf_bc")
    dma(identb, identb_d, writes=[B_const])
    dma(identf, identf_d, partial=[B_const])
    dma(perm64[0:64, :], perm_d, partial=[B_const])
    dma(esink, sink_d, partial=[B_const])
    B_c2 = Buf("const2")
    vop("pool", "memset", writes=[B_c2], ap=onesb, constant=1.0)
    vop("pool", "memset", partial=[B_c2], ap=onesf, constant=1.0)
    vop("pool", "memset", partial=[B_c2], ap=epsb, constant=EPS)
    B_es = Buf("esink")
    act(esink, esink, AF.Exp, reads=[B_const], writes=[B_es])
    S.barrier()
    persist_mark = cv.mark()

    def phase0():
        m0 = cv.mark()
        cT = cv.alloc([32], F32)
        csil = cv.alloc([32], F32)
        gcol = cv.alloc([32], F32)
        modrow = cv.alloc([6 * D], F32)
        bmod = cv.alloc([6 * D], F32)
        modcol = cv.alloc([96, 2], F32)
        NST = 2
        stg = [cv.alloc([16, 512], F32) for _ in range(NST)]
        Bst = [Buf("stg%d" % i) for i in range(NST)]
        B_cT, B_cs, B_gc, B_mr, B_bm, B_mc = (Buf(n) for n in ("cT", "csil", "gcol", "modrow", "bmod", "modcol"))
        dma(cT, cT_d, writes=[B_cT])
        dma(gcol, gcol_d, writes=[B_gc])
        dma(bmod[0:2, :], b_mod2, writes=[B_bm])
        act(csil, cT, AF.Silu, reads=[B_cT], writes=[B_cs])
        csil3 = csil.rearrange("p (k j) -> p k j", j=2)
        wm = w_mod.rearrange("(k p) n -> p k n", p=128)
        for u in range(24):
            sl = u % NST
            for q in range(4):
                dma(stg[sl][:, q * 4:(q + 1) * 4, :], wm[:, q * 4:(q + 1) * 4, u * 512:(u + 1) * 512],
                    **({"writes": [Bst[sl]]} if q == 0 else {"partial": [Bst[sl]]}))
            bank = u % 2
            for k in range(16):
                mm(ps[bank][0:2, :], csil3[:, k, :], stg[sl][:, k, :], k == 0, k == 15,
                   reads=[B_cs, Bst[sl]], partial=[PB[bank]])
            vop("dve", "tensor_tensor", reads=[PB[bank], B_bm], partial=[B_mr],
                out=modrow[0:2, u * 512:(u + 1) * 512], in0=ps[bank][0:2, :], in1=bmod[0:2, u * 512:(u + 1) * 512],
                op=ALU.add)
        pcol = ps[2][:, 0:192].rearrange("p (j t) -> p j t", t=2)
        for j in range(96):
            tr(pcol[:, j, :], modrow[0:2, j * 128:(j + 1) * 128], identf[0:2, 0:2], reads=[B_mr, B_const], partial=[PB[2]])
        vop("dve", "tensor_copy", reads=[PB[2]], writes=[B_mc], out=modcol, in_=pcol)
        one = 1.0
        vop("dve", "scalar_tensor_tensor", reads=[B_mc, B_gc], writes=[B_cols], out=cols[:, 0, :], in0=modcol[:, 16:32, 0],
            scalar=one, in1=gcol[:, 0:16], op0=ALU.add, op1=ALU.mult)
        vop("dve", "tensor_copy", reads=[B_mc], partial=[B_cols], out=cols[:, 1, :], in_=modcol[:, 0:16, 0])
        vop("dve", "scalar_tensor_tensor", reads=[B_mc, B_gc], partial=[B_cols], out=cols[:, 2, :], in0=modcol[:, 16:32, 1],
            scalar=one, in1=gcol[:, 0:16], op0=ALU.add, op1=ALU.mult)
        vop("dve", "tensor_copy", reads=[B_mc], partial=[B_cols], out=cols[:, 3, :], in_=modcol[:, 0:16, 1])
        vop("dve", "scalar_tensor_tensor", reads=[B_mc, B_gc], partial=[B_cols], out=cols[:, 4, :], in0=modcol[:, 64:80, 0],
            scalar=one, in1=gcol[:, 16:32], op0=ALU.add, op1=ALU.mult)
        vop("dve", "tensor_copy", reads=[B_mc], partial=[B_cols], out=cols[:, 5, :], in_=modcol[:, 48:64, 0])
        for (v, dst, Bd) in ((2, ga_bc, B_ga), (5, gf_bc, B_gf)):
            for n in range(4):
                bank = 3 + (n % 2)
                mm(ps[bank][:, :], onesf[0:1, :], modrow[0:1, v * D + n * 512: v * D + (n + 1) * 512], True, True,
                   reads=[B_mr, B_c2], writes=[PB[bank]])
                vop("dve", "tensor_copy", reads=[PB[bank]], partial=[Bd], out=dst[:, n * 512:(n + 1) * 512], in_=ps[bank][:, :])
        if dbg:
            d1 = dbg_out("dbg_cols", [128, 96])
            dma(d1, cols.rearrange("p a b -> p (a b)"), reads=[B_cols])
            d2 = dbg_out("dbg_ga", [128, D])
            dma(d2, ga_bc, reads=[B_ga])
            d3 = dbg_out("dbg_gf", [128, D])
            dma(d3, gf_bc, reads=[B_gf])
        S.barrier()
        cv.release(m0)

    phase0()

    class HT:
        def __init__(self, banks=(0, 1), nxt=2):
            self.nxt = nxt
            self.xt = [cv.alloc([D], F32) for _ in range(nxt)]
            self.Bxt = [Buf("xt%d" % i) for i in range(nxt)]
            self.ssq = cv.alloc([8], F32)
            self.rstd = cv.alloc([8], F32)
            self.Bss = [Buf("ssq%d" % i) for i in range(4)]
            self.Brs = [Buf("rstd%d" % i) for i in range(4)]
            self.xn = cv.alloc([4, D], BF16)
            self.Bxn = [Buf("xn%d" % i) for i in range(4)]
            self.hT = cv.alloc([16, 512], BF16)
            self.BhT = Buf("hT")
            self.banks = banks
            self.cnt = 0

        def run(self, srcs, segs):
            nt = len(srcs)
            for tt, src in enumerate(srcs):
                sl = self.cnt % self.nxt
                self.cnt += 1
                dma(self.xt[sl], src, writes=[self.Bxt[sl]])
                act(self.xn[:, tt, :], self.xt[sl], AF.Square, reads=[self.Bxt[sl]], writes=[self.Bss[tt], self.Bxn[tt]],
                    accum_out=self.ssq[:, tt:tt + 1])
                act(self.rstd[:, tt:tt + 1], self.ssq[:, tt:tt + 1], AF.Sqrt, reads=[self.Bss[tt], B_c2], writes=[self.Brs[tt]],
                    scale=1.0 / D, bias=epsb[:, 0:1])
                vop("dve", "reciprocal", reads=[self.Brs[tt]], writes=[self.Brs[tt]],
                    out=self.rstd[:, tt:tt + 1], in_=self.rstd[:, tt:tt + 1])
                vop("pool", "tensor_scalar", reads=[self.Bxt[sl], self.Brs[tt]], writes=[self.Bxn[tt]],
                    out=self.xn[:, tt, :], in0=self.xt[sl], scalar1=self.rstd[:, tt:tt + 1], scalar2=None, op0=ALU.mult)
            for kk in range(8):
                bank = self.banks[kk % 2]
                pv = psb(bank).rearrange("p (a t c) -> p a t c", a=2, t=4)
                for a in range(2):
                    k = kk * 2 + a
                    for tt in range(nt):
                        tr(pv[:, a, tt, :], self.xn[:, tt, k * 128:(k + 1) * 128], identb,
                           reads=[self.Bxn[tt], B_const], partial=[PB[bank]])
                for a in range(2):
                    k = kk * 2 + a
                    for (lo, hi, gi, si) in segs:
                        src = pv[:, a, lo:hi, :]
                        dst = self.hT[:, k, lo * 128:hi * 128].rearrange("p (t c) -> p t c", c=128)
                        if True:
                            act(dst, src, AF.Identity, reads=[PB[bank], B_cols], partial=[self.BhT],
                                scale=cols[:, gi, k:k + 1], bias=cols[:, si, k:k + 1])
                        else:
                            vop("dve", "tensor_scalar", reads=[PB[bank], B_cols], partial=[self.BhT],
                                out=dst, in0=src, scalar1=cols[:, gi, k:k + 1], scalar2=cols[:, si, k:k + 1],
                                op0=ALU.mult, op1=ALU.add)
            return self.hT, self.BhT

    class Rope:
        def __init__(self, bank_sw, nb=2):
            self.nb = nb
            self.qsb = [cv.alloc([512], F32) for _ in range(nb)]
            self.t1 = [cv.alloc([512], F32) for _ in range(nb)]
            self.t2 = [cv.alloc([512], F32) for _ in range(nb)]
            self.Bq = [Buf("rq%d" % i) for i in range(nb)]
            self.B1 = [Buf("rt1%d" % i) for i in range(nb)]
            self.B2 = [Buf("rt2%d" % i) for i in range(nb)]
            self.tabC = [cv.alloc([512], F32) for _ in range(nb)]
            self.tabS = [cv.alloc([512], F32) for _ in range(nb)]
            self.Btab = [Buf("tab%d" % i) for i in range(nb)]
            self.bank_sw = bank_sw
            self.cnt = 0
            self.tcnt = 0
            self.cur = 0

        def load_tables(self, pieces):
            sl = self.tcnt % self.nb
            self.tcnt += 1
            self.cur = sl
            first = True
            for (dc, sc, n) in pieces:
                kw = {"writes": [self.Btab[sl]]} if first else {"partial": [self.Btab[sl]]}
                dma(self.tabC[sl][0:64, dc:dc + n], ropeC_d[:, sc:sc + n], **kw)
                dma(self.tabS[sl][0:64, dc:dc + n], ropeS_d[:, sc:sc + n], partial=[self.Btab[sl]])
                first = False

        def apply(self, psrc, Bsrc, dst, Bdst, c0, n, dst_partial=True):
            i = self.cnt % self.nb
            self.cnt += 1
            sl = self.cur
            bsw = self.bank_sw
            act(self.qsb[i][0:64, 0:n], psrc, AF.Copy, reads=[Bsrc], writes=[self.Bq[i]])
            mm(ps[bsw][0:64, 0:n], perm64[0:64, :], self.qsb[i][0:64, 0:n], True, True,
               reads=[self.Bq[i], B_const], writes=[PB[bsw]])
            vop("dve", "tensor_tensor", reads=[self.Bq[i], self.Btab[sl]], writes=[self.B1[i]],
                out=self.t1[i][0:64, 0:n], in0=self.qsb[i][0:64, 0:n], in1=self.tabC[sl][0:64, c0:c0 + n], op=ALU.mult)
            vop("dve", "tensor_tensor", reads=[PB[bsw], self.Btab[sl]], writes=[self.B2[i]],
                out=self.t2[i][0:64, 0:n], in0=ps[bsw][0:64, 0:n], in1=self.tabS[sl][0:64, c0:c0 + n], op=ALU.mult)
            kw = {"partial": [Bdst]} if dst_partial else {"writes": [Bdst]}
            vop("pool", "tensor_tensor", reads=[self.B1[i], self.B2[i]],
                out=dst, in0=self.t1[i][0:64, 0:n], in1=self.t2[i][0:64, 0:n], op=ALU.add, **kw)

    def load_cast(dst_bf, Bdst, src_ap, stg, Bstg, shape_a, eng_cast, partial=False, split=4):
        a = shape_a
        step = max(1, a // split)
        first = True
        for q in range(0, a, step):
            kw = {"writes": [Bstg]} if first else {"partial": [Bstg]}
            dma(stg[:, q:q + step, :], src_ap[:, q:q + step, :], **kw)
            first = False
        kw = {"partial": [Bdst]} if partial else {"writes": [Bdst]}
        if eng_cast == "act":
            act(dst_bf, stg, AF.Copy, reads=[Bstg], **kw)
        else:
            vop(eng_cast, "tensor_copy", reads=[Bstg], out=dst_bf, in_=stg, **kw)

    win = w_in.rearrange("(k p) n -> p k n", p=128)

    def phaseP():
        m0 = cv.mark()
        NS = 3
        stg = [cv.alloc([8192], F32) for _ in range(NS)]
        obf = [cv.alloc([8192], BF16) for _ in range(NS)]
        Bs = [Buf("pstg%d" % i) for i in range(NS)]
        Bo = [Buf("pobf%d" % i) for i in range(NS)]
        units = []
        for e in range(65):
            if e < 64:
                g, u, dn = w_gate[e], w_up[e], w_down[e]
            else:
                g, u, dn = ws_gate, ws_up, ws_down
            units.append((g.rearrange("(k p) f -> p k f", p=128), 16, 128, wexp_d[0][e]))
            units.append((u.rearrange("(k p) f -> p k f", p=128), 16, 128, wexp_d[1][e]))
            units.append((dn.rearrange("(k p) f -> p k f", p=128), 4, 128, wexp_d[2][e]))
        for n in range(4):
            units.append((w_out[0:1024, n * 512:(n + 1) * 512].rearrange("(h p) f -> p h f", p=64), 16, 64, woutA_d[n]))
            units.append((w_out[1024:2048, n * 512:(n + 1) * 512].rearrange("(k p) f -> p k f", p=128), 8, 128, woutB_d[n]))
        cast_engs = ("act", "dve", "pool")
        for i, (src, a, npart, dst) in enumerate(units):
            sl = i % NS
            b = src.shape[2]
            sv = stg[sl][0:npart, 0:a * b].rearrange("p (a b) -> p a b", a=a)
            ov = obf[sl][0:npart, 0:a * b]
            step = max(1, a // 4)
            first = True
            for q in range(0, a, step):
                kw = {"writes": [Bs[sl]]} if first else {"partial": [Bs[sl]]}
                dma(sv[:, q:q + step, :], src[:, q:q + step, :], **kw)
                first = False
            ce = cast_engs[i % 3]
            svf = stg[sl][0:npart, 0:a * b]
            if ce == "act":
                act(ov, svf, AF.Copy, reads=[Bs[sl]], writes=[Bo[sl]])
            else:
                vop(ce, "tensor_copy", reads=[Bs[sl]], writes=[Bo[sl]], out=ov, in_=svf)
            dma(dst, ov, reads=[Bo[sl]], eng="pool", sem_buf=Bo[sl])
        S.barrier()
        cv.release(m0)

    if stage >= 4:
        phaseP()

    def progB():
        m0 = cv.mark()
        ckvnT = cv.alloc([2, SEQ + CTX], BF16)
        krT = cv.alloc([SEQ + CTX], BF16)
        cqnT = cv.alloc([4, OWN], BF16)
        B_ckvnT, B_krT, B_cqnT = Buf("ckvnT"), Buf("krT"), Buf("cqnT")
        m1 = cv.mark()
        Wcq = cv.alloc([16, 512], BF16)
        Wckv = cv.alloc([16, 256], BF16)
        Wkr = cv.alloc([16, 64], BF16)
        BW = Buf("WB")
        gq = cv.alloc([512], F32)
        gkv = cv.alloc([256], F32)
        Bg = Buf("gqkv")
        dma(gq, gq_d, writes=[Bg])
        dma(gkv, gkv_d, partial=[Bg])
        ms = cv.mark()
        stg = cv.alloc([16, 512], F32)
        Bstg = Buf("stgB")
        load_cast(Wcq, BW, win[:, :, 1536:2048], stg, Bstg, 16, "dve")
        load_cast(Wckv, BW, win[:, :, 2048:2304], stg[:, :, 0:256], Bstg, 16, "dve", partial=True)
        load_cast(Wkr, BW, win[:, :, 2304:2368], stg[:, :, 0:64], Bstg, 16, "dve", partial=True)
        S.barrier()
        cv.release(ms)
        ht = HT(banks=(0, 1))
        rope = Rope(bank_sw=7)
        ssq2 = cv.alloc([8], F32)
        rs2 = cv.alloc([8], F32)
        Bs2 = [Buf("ssq2_%d" % i) for i in range(8)]
        Br2 = [Buf("rs2_%d" % i) for i in range(8)]
        ckvn = [cv.alloc([256], BF16) for _ in range(2)]
        cqn = [cv.alloc([512], BF16) for _ in range(2)]
        Bckvn = [Buf("ckvn0"), Buf("ckvn1")]
        Bcqn = [Buf("cqn0"), Buf("cqn1")]
        junk2 = cv.alloc([512], BF16)
        cnt = 0
        import os as _os
        for st in range(int(_os.environ.get('NST', '17')) if bis >= 1 else 0):
            if st < 16:
                srcs = [xs[st * 512 + tt * 128: st * 512 + (tt + 1) * 128, :] for tt in range(4)]
                segs = [(0, 4, 0, 1)]
            else:
                srcs = [ctxb[tt * 128:(tt + 1) * 128, :] for tt in range(2)]
                segs = [(0, 2, 2, 3)]
            nt = len(srcs)
            ntok = nt * 128
            tok0 = st * 512
            hT, BhT = ht.run(srcs, segs)
            if bis < 2:
                continue
            if st < 16:
                rope.load_tables([(0, tok0, 512)])
            for tt in range(nt):
                j = cnt % 2
                cnt += 1
                for k in range(16):
                    mm(ps[2][:, 0:256], hT[:, k, tt * 128:(tt + 1) * 128], Wckv[:, k, :], k == 0, k == 15,
                       reads=[BhT, BW], partial=[PB[2]])
                act(junk2[:, 0:256], ps[2][:, 0:256], AF.Square, reads=[PB[2]], writes=[Bs2[j]], accum_out=ssq2[:, j:j + 1])
                act(rs2[:, j:j + 1], ssq2[:, j:j + 1], AF.Sqrt, reads=[Bs2[j], B_c2], writes=[Br2[j]], scale=1.0 / 256, bias=epsb[:, 0:1])
                vop("dve", "reciprocal", reads=[Br2[j]], writes=[Br2[j]], out=rs2[:, j:j + 1], in_=rs2[:, j:j + 1])
                vop("dve", "scalar_tensor_tensor", reads=[PB[2], Br2[j], Bg], writes=[Bckvn[j]],
                    out=ckvn[j], in0=ps[2][:, 0:256], scalar=rs2[:, j:j + 1], in1=gkv, op0=ALU.mult, op1=ALU.mult)
                pv4 = psb(4)[:, 0:256].rearrange("p (c t) -> p c t", c=2)
                for c in range(2):
                    tr(pv4[:, c, :], ckvn[j][:, c * 128:(c + 1) * 128], identb, reads=[Bckvn[j], B_const], partial=[PB[4]])
                act(ckvnT[:, :, tok0 + tt * 128: tok0 + (tt + 1) * 128], pv4, AF.Copy, reads=[PB[4]], partial=[B_ckvnT])
                if st < 8 and bis >= 3:
                    for k in range(16):
                        mm(ps[3][:, :], hT[:, k, tt * 128:(tt + 1) * 128], Wcq[:, k, :], k == 0, k == 15,
                           reads=[BhT, BW], partial=[PB[3]])
                    jj = 4 + j
                    act(junk2, ps[3][:, :], AF.Square, reads=[PB[3]], writes=[Bs2[jj]], accum_out=ssq2[:, jj:jj + 1])
                    act(rs2[:, jj:jj + 1], ssq2[:, jj:jj + 1], AF.Sqrt, reads=[Bs2[jj], B_c2], writes=[Br2[jj]], scale=1.0 / 512, bias=epsb[:, 0:1])
                    vop("dve", "reciprocal", reads=[Br2[jj]], writes=[Br2[jj]], out=rs2[:, jj:jj + 1], in_=rs2[:, jj:jj + 1])
                    vop("dve", "scalar_tensor_tensor", reads=[PB[3], Br2[jj], Bg], writes=[Bcqn[j]],
                        out=cqn[j], in0=ps[3][:, :], scalar=rs2[:, jj:jj + 1], in1=gq, op0=ALU.mult, op1=ALU.mult)
                    pv5 = psb(5)[:, 0:512].rearrange("p (c t) -> p c t", c=4)
                    for c in range(4):
                        tr(pv5[:, c, :], cqn[j][:, c * 128:(c + 1) * 128], identb, reads=[Bcqn[j], B_const], partial=[PB[5]])
                    vop("dve", "tensor_copy", reads=[PB[5]], partial=[B_cqnT],
                        out=cqnT[:, :, tok0 + tt * 128: tok0 + (tt + 1) * 128], in_=pv5)
            for k in range(16):
                mm(ps[6][0:64, 0:ntok], Wkr[:, k, :], hT[:, k, 0:ntok], k == 0, k == 15, reads=[BhT, BW], partial=[PB[6]])
            if st < 16 and bis >= 4:
                rope.apply(ps[6][0:64, 0:ntok], PB[6], krT[0:64, tok0:tok0 + ntok], B_krT, 0, ntok)
            else:
                act(krT[0:64, tok0:tok0 + ntok], ps[6][0:64, 0:ntok], AF.Copy, reads=[PB[6]], partial=[B_krT])
        if dbg and stage == 1:
            dma(dbg_out("dbg_ckvnT", [128, 2 * (SEQ + CTX)], BF16), ckvnT.rearrange("p a b -> p (a b)"), reads=[B_ckvnT])
            dma(dbg_out("dbg_krT", [64, SEQ + CTX], BF16), krT[0:64, :], reads=[B_krT])
            dma(dbg_out("dbg_cqnT", [128, 4 * OWN], BF16), cqnT.rearrange("p a b -> p (a b)"), reads=[B_cqnT])
        S.barrier()
        cv.release(m1)
        if stage < 2:
            cv.release(m0)
            return
        Wuq = cv.alloc([4, 1536], BF16)
        Wukv = cv.alloc([2, 2048], BF16)
        BWu = Buf("Wu")
        ms = cv.mark()
        stg2 = cv.alloc([4, 1536], F32)
        Bstg2 = Buf("stg2")
        load_cast(Wuq, BWu, w_uq.rearrange("(k p) n -> p k n", p=128), stg2, Bstg2, 4, "dve")
        load_cast(Wukv, BWu, w_ukv.rearrange("(k p) n -> p k n", p=128),
                  stg2.rearrange("p a b -> p (a b)")[:, 0:4096].rearrange("p (a b) -> p a b", a=2), Bstg2, 2, "dve", partial=True)
        S.barrier()
        cv.release(ms)
        KhT = cv.alloc([SEQ + CTX], BF16)
        Vh = cv.alloc([66, 128], BF16)
        qnT = cv.alloc([OWN], BF16)
        qrT = cv.alloc([OWN], BF16)
        B_K, B_V, B_qn, B_qr = Buf("KhT"), Buf("Vh"), Buf("qnT"), Buf("qrT")
        rope = Rope(bank_sw=7)
        NPT = 3
        pT = [cv.alloc([512], BF16) for _ in range(NPT)]
        BpT = [Buf("pT%d" % i) for i in range(NPT)]
        rec = cv.alloc([512], F32)
        Brec = Buf("rec")
        ot = [cv.alloc([512], BF16) for _ in range(2)]
        Bot = [Buf("ot0"), Buf("ot1")]
        scale = 192 ** -0.5
        NKT = 66
        ocnt = 0
        for h in range(8):
            for kc in range(17):
                n = 512 if kc < 16 else 256
                bank = kc % 2
                for c in range(2):
                    mm(ps[bank][:, 0:n], Wukv[:, c, h * 256: h * 256 + 128], ckvnT[:, c, kc * 512: kc * 512 + n], c == 0, c == 1,
                       reads=[BWu, B_ckvnT], partial=[PB[bank]])
                if kc % 2 == 0:
                    act(KhT[:, kc * 512: kc * 512 + n], ps[bank][:, 0:n], AF.Copy, reads=[PB[bank]], partial=[B_K])
                else:
                    vop("dve", "tensor_copy", reads=[PB[bank]], partial=[B_K], out=KhT[:, kc * 512: kc * 512 + n], in_=ps[bank][:, 0:n])
            for gt in range(17):
                ntl = 4 if gt < 16 else 2
                bank = 2 + gt % 2
                for j in range(ntl):
                    tile = gt * 4 + j
                    for c in range(2):
                        mm(ps[bank][:, j * 128:(j + 1) * 128], ckvnT[:, c, tile * 128:(tile + 1) * 128],
                           Wukv[:, c, h * 256 + 128: h * 256 + 256], c == 0, c == 1, reads=[BWu, B_ckvnT], partial=[PB[bank]])
                dstv = Vh[:, gt * 4: gt * 4 + ntl, :]
                srcv = ps[bank][:, 0:ntl * 128].rearrange("p (t c) -> p t c", c=128)
                if gt % 2 == 0:
                    vop("dve", "tensor_copy", reads=[PB[bank]], partial=[B_V], out=dstv, in_=srcv)
                else:
                    act(dstv, srcv, AF.Copy, reads=[PB[bank]], partial=[B_V])
            for qc in range(8):
                bank = 4 + qc % 2
                for c in range(4):
                    mm(ps[bank][:, :], Wuq[:, c, h * 192: h * 192 + 128], cqnT[:, c, qc * 512:(qc + 1) * 512], c == 0, c == 3,
                       reads=[BWu, B_cqnT], partial=[PB[bank]])
                vop("dve", "tensor_copy", reads=[PB[bank]], partial=[B_qn], out=qnT[:, qc * 512:(qc + 1) * 512], in_=ps[bank][:, :])
                for c in range(4):
                    mm(ps[6][0:64, :], Wuq[:, c, h * 192 + 128: h * 192 + 192], cqnT[:, c, qc * 512:(qc + 1) * 512], c == 0, c == 3,
                       reads=[BWu, B_cqnT], partial=[PB[6]])
                rope.load_tables([(0, qc * 512, 512)])
                rope.apply(ps[6][0:64, :], PB[6], qrT[0:64, qc * 512:(qc + 1) * 512], B_qr, 0, 512)
            for qc in range(8):
                bO = 4 + qc % 2
                bD = 6 + qc % 2
                qs = slice(qc * 512, (qc + 1) * 512)

                def s_mm(kt):
                    b = kt % 3
                    mm(ps[b][:, :], KhT[:, kt * 128:(kt + 1) * 128], qnT[:, qs], True, False,
                       reads=[B_K, B_qn], partial=[PB[b]])
                    mm(ps[b][:, :], krT[0:64, kt * 128:(kt + 1) * 128], qrT[0:64, qs], False, True,
                       reads=[B_krT, B_qr], partial=[PB[b]])
                    act(pT[b], ps[b][:, :], AF.Exp, reads=[PB[b]], writes=[BpT[b]], scale=scale)

                def pv_mm(kt):
                    b = kt % 3
                    mm(ps[bO][:, :], Vh[:, kt, :], pT[b], kt == 0, kt == NKT - 1, reads=[B_V, BpT[b]], partial=[PB[bO]])
                    mm(ps[bD][:, :], onesb, pT[b], kt == 0, kt == NKT - 1, reads=[B_c2, BpT[b]], partial=[PB[bD]])

                s_mm(0)
                s_mm(1)
                for kt in range(NKT):
                    if kt + 2 < NKT:
                        s_mm(kt + 2)
                    pv_mm(kt)
                vop("dve", "reciprocal", reads=[PB[bD]], writes=[Brec], out=rec, in_=ps[bD][:, :])
                oi = ocnt % 2
                ocnt += 1
                vop("dve", "tensor_tensor", reads=[PB[bO], Brec], writes=[Bot[oi]], out=ot[oi], in0=ps[bO][:, :], in1=rec, op=ALU.mult)
                dma(OTb_d[h, :, qs], ot[oi], reads=[Bot[oi]], eng="pool", sem_buf=Bot[oi])
        S.barrier()
        if dbg and stage == 2:
            dma(dbg_out("dbg_OTb", [8, 128, OWN], BF16), OTb_d, reads=[Buf("x")])
            S.barrier()
        cv.release(m0)

    if stage >= 1:
        progB()

    def progA():
        m0 = cv.mark()
        WA = cv.alloc([16, 1536], BF16)
        BWA = Buf("WA")
        ms = cv.mark()
        stg = cv.alloc([16, 512], F32)
        Bstg = Buf("stgA")
        for i in range(3):
            load_cast(WA[:, :, i * 512:(i + 1) * 512], BWA, win[:, :, i * 512:(i + 1) * 512], stg, Bstg, 16,
                      ("dve", "act", "dve")[i], partial=(i > 0))
        S.barrier()
        cv.release(ms)
        NSL = 36
        kaT = cv.alloc([4, NSL * 128], BF16)
        va = cv.alloc([NSL, 256], BF16)
        qaT = cv.alloc([16, 512], BF16)
        B_ka = [Buf("kaT%d" % i) for i in range(10)]
        B_va = [Buf("va%d" % i) for i in range(10)]
        B_qa = Buf("qaT")
        msk = cv.alloc([8, 512], BF16)
        B_msk = Buf("msk")
        dma(msk, masks_d.rearrange("j p c -> p j c"), writes=[B_msk])
        ht = HT(banks=(0, 1), nxt=1)
        rope = Rope(bank_sw=7, nb=1)
        NPT = 3
        pT = [cv.alloc([512], BF16) for _ in range(NPT)]
        BpT = [Buf("pTa%d" % i) for i in range(NPT)]
        rec = cv.alloc([512], F32)
        Brec = Buf("recA")
        ot = [cv.alloc([512], BF16) for _ in range(2)]
        Bot = [Buf("otA0"), Buf("otA1")]
        state = {"o": 0, "p": 0}

        def kside(srcs, segs, slot0, grp, rope_cols, tabs):
            nt = len(srcs)
            hT, BhT = ht.run(srcs, segs)
            if tabs:
                rope.load_tables(tabs)
            for hk in range(4):
                for k in range(16):
                    mm(ps[2][0:64, :], WA[:, k, 1024 + hk * 64: 1024 + (hk + 1) * 64], hT[:, k, :], k == 0, k == 15,
                       reads=[BhT, BWA], partial=[PB[2]])
                dst = kaT[0:64, hk, slot0 * 128: slot0 * 128 + 512]
                if rope_cols > 0:
                    rope.apply(ps[2][0:64, 0:rope_cols], PB[2], dst[:, 0:rope_cols], B_ka[grp], 0, rope_cols)
                if rope_cols < 512:
                    act(dst[:, rope_cols:512], ps[2][0:64, rope_cols:512], AF.Copy, reads=[PB[2]], partial=[B_ka[grp]])
            for tt in range(nt):
                bank = 3 + tt % 2
                for k in range(16):
                    mm(ps[bank][:, 0:256], hT[:, k, tt * 128:(tt + 1) * 128], WA[:, k, 1280:1536], k == 0, k == 15,
                       reads=[BhT, BWA], partial=[PB[bank]])
                vop("dve", "tensor_copy", reads=[PB[bank]], partial=[B_va[grp]], out=va[:, slot0 + tt, :], in_=ps[bank][:, 0:256])
            return hT, BhT

        def qside(hT, BhT):
            for h in range(16):
                bank = 5 + h % 2
                for k in range(16):
                    mm(ps[bank][0:64, :], WA[:, k, h * 64:(h + 1) * 64], hT[:, k, :], k == 0, k == 15,
                       reads=[BhT, BWA], partial=[PB[bank]])
                rope.apply(ps[bank][0:64, :], PB[bank], qaT[0:64, h, :], B_qa, 0, 512)

        def attend(s):
            prev = (4 * s - 1, 0, s - 1) if s > 0 else (33, 6, 8)
            nxt = (4 * s + 4, 5, s + 1) if s < 7 else (32, 7, 8)
            tiles = [prev] + [(4 * s + j, 1 + j, s) for j in range(4)] + [nxt, (34, None, 8), (35, None, 8)]
            for h in range(16):
                hk = h // 4
                bO = 3 + h % 2
                bD = 5 + h % 2
                n = len(tiles)
                for i, (slot, mi, grp) in enumerate(tiles):
                    b = state["p"] % NPT
                    state["p"] += 1
                    mm(ps[b][:, :], kaT[0:64, hk, slot * 128:(slot + 1) * 128], qaT[0:64, h, :], True, True,
                       reads=[B_ka[grp], B_qa], writes=[PB[b]])
                    act(pT[b], ps[b][:, :], AF.Exp, reads=[PB[b]], writes=[BpT[b]], scale=0.125)
                    if mi is not None:
                        vop("pool", "tensor_tensor", reads=[BpT[b], B_msk], writes=[BpT[b]],
                            out=pT[b], in0=pT[b], in1=msk[:, mi, :], op=ALU.mult)
                    mm(ps[bO][0:64, :], va[:, slot, hk * 64:(hk + 1) * 64], pT[b], i == 0, i == n - 1,
                       reads=[B_va[grp], BpT[b]], partial=[PB[bO]])
                    mm(ps[bD][0:64, :], onesb[:, 0:64], pT[b], i == 0, i == n - 1, reads=[B_c2, BpT[b]], partial=[PB[bD]])
                vop("dve", "tensor_scalar", reads=[PB[bD], B_es], writes=[Brec], out=rec[0:64, :], in0=ps[bD][0:64, :],
                    scalar1=esink[0:64, h:h + 1], scalar2=None, op0=ALU.add)
                vop("dve", "reciprocal", reads=[Brec], writes=[Brec], out=rec[0:64, :], in_=rec[0:64, :])
                oi = state["o"] % 2
                state["o"] += 1
                vop("dve", "tensor_tensor", reads=[PB[bO], Brec], writes=[Bot[oi]], out=ot[oi][0:64, :], in0=ps[bO][0:64, :],
                    in1=rec[0:64, :], op=ALU.mult)
                dma(OTa_d[h, :, s * 512:(s + 1) * 512], ot[oi][0:64, :], reads=[Bot[oi]], eng="pool", sem_buf=Bot[oi])

        srcs = [xs[32 * 128:33 * 128, :], xs[63 * 128:64 * 128, :], ctxb[0:128, :], ctxb[128:256, :]]
        kside(srcs, [(0, 2, 0, 1), (2, 4, 2, 3)], 32, 8, 256, [(0, 32 * 128, 128), (128, 63 * 128, 128)])
        for s in range(8):
            srcs = [xs[s * 512 + tt * 128: s * 512 + (tt + 1) * 128, :] for tt in range(4)]
            hT, BhT = kside(srcs, [(0, 4, 0, 1)], 4 * s, s, 512, [(0, s * 512, 512)])
            if s > 0:
                attend(s - 1)
            qside(hT, BhT)
        attend(7)
        S.barrier()
        if dbg and stage == 3:
            dma(dbg_out("dbg_OTa", [16, 64, OWN], BF16), OTa_d, reads=[Buf("x")])
            S.barrier()
        cv.release(m0)

    if stage >= 3:
        progA()

    def phase5():
        m0 = cv.mark()
        xnew_d = dscr("xnew", [OWN, D], F32)
        gfin = cv.alloc([D], F32)
        rbias = cv.alloc([NE], F32)
        wr32 = cv.alloc([16, NE], F32)
        Bc5 = Buf("c5")
        dma(gfin, gfin_d, writes=[Bc5])
        dma(rbias, rbias_d, partial=[Bc5])
        dma(wr32, w_router.rearrange("(k p) e -> p k e", p=128), partial=[Bc5])
        xt = cv.alloc([4, D], F32)
        Bxt = [Buf("x5_%d" % i) for i in range(4)]
        yacc = cv.alloc([4, D], F32)
        yoff = cv.last_off
        By = [[Buf("y%d_%d" % (t, n)) for n in range(4)] for t in range(4)]
        OTa = cv.view(yoff, [16, 512], BF16)
        OTb = cv.view(yoff + 16384, [8, 512], BF16)
        h2T = cv.alloc([16, 512], BF16)
        Bh2 = Buf("h2T")
        ring = []
        roff = []
        for i in range(4):
            ring.append(cv.alloc([8192], BF16))
            roff.append(cv.last_off)
        BR = [Buf("ring%d" % i) for i in range(4)]
        h2T32 = cv.view(roff[2], [16, 512], F32)
        actT = [cv.alloc([4, 512], BF16) for _ in range(2)]
        BaT = [Buf("actT0"), Buf("actT1")]
        stmp = [cv.alloc([512], F32) for _ in range(2)]
        Bst = [Buf("stmp0"), Buf("stmp1")]
        G = cv.alloc([4, NE + 1], F32)
        BG = [Buf("G%d" % i) for i in range(4)]
        sm = cv.alloc([512], F32)
        Bsm = Buf("sm")
        ssq = cv.alloc([8], F32)
        rstd = cv.alloc([8], F32)
        Bss = [Buf("ss5_%d" % i) for i in range(8)]
        Brs = [Buf("rs5_%d" % i) for i in range(8)]
        for tt in range(4):
            vop("pool", "memset", writes=[BG[tt]], ap=G[:, tt, NE:NE + 1], constant=1.0)
        sg, sel, selm, ch, wv = (sm[:, i * 64:(i + 1) * 64] for i in range(5))
        gs8 = sm[:, 320:384].rearrange("p (g j) -> p g j", j=8)
        gsc, top8, gm, pen, top8b = (sm[:, 384 + i * 8: 392 + i * 8] for i in range(5))
        wsum = sm[:, 424:425]
        rws = sm[:, 425:426]
        ucnt = {"n": 0}

        def load_unit(src, npart, ncols):
            sl = ucnt["n"] % 4
            ucnt["n"] += 1
            half = ncols // 2
            dma(ring[sl][0:npart, 0:half], src[:, 0:half], writes=[BR[sl]])
            dma(ring[sl][0:npart, half:ncols], src[:, half:ncols], partial=[BR[sl]])
            return sl

        for s in range(8):
            tok0 = s * 512
            for tt in range(4):
                dma(xt[:, tt, :], xs[tok0 + tt * 128: tok0 + (tt + 1) * 128, :], writes=[Bxt[tt]])
            allY = [By[t][n] for t in range(4) for n in range(4)]
            dma(OTa[0:64, :, :], OTa_d[:, :, tok0:tok0 + 512].rearrange("h p t -> p h t"), writes=allY)
            dma(OTb, OTb_d[:, :, tok0:tok0 + 512].rearrange("h p t -> p h t"), partial=allY)
            ocnt = 0
            for n in range(4):
                sa = load_unit(woutA_d[n], 64, 8192)
                sb_ = load_unit(woutB_d[n], 128, 4096)
                WAv = ring[sa].rearrange("p (h f) -> p h f", h=16)
                WBv = ring[sb_][:, 0:4096].rearrange("p (k f) -> p k f", k=8)
                for tt in range(4):
                    bank = ocnt % 2
                    i2 = ocnt % 2
                    ocnt += 1
                    for h in range(16):
                        mm(ps[bank][:, :], OTa[0:64, h, tt * 128:(tt + 1) * 128], WAv[0:64, h, :], h == 0, False,
                           reads=allY + [BR[sa]], partial=[PB[bank]])
                    for k in range(8):
                        mm(ps[bank][:, :], OTb[:, k, tt * 128:(tt + 1) * 128], WBv[:, k, :], False, k == 7,
                           reads=allY + [BR[sb_]], partial=[PB[bank]])
                    vop("dve", "tensor_tensor", reads=[PB[bank], B_ga], writes=[Bst[i2]], out=stmp[i2], in0=ps[bank][:, :],
                        in1=ga_bc[:, n * 512:(n + 1) * 512], op=ALU.mult)
                    vop("pool", "tensor_tensor", reads=[Bst[i2]], partial=[Bxt[tt]], out=xt[:, tt, n * 512:(n + 1) * 512],
                        in0=xt[:, tt, n * 512:(n + 1) * 512], in1=stmp[i2], op=ALU.add)
            junk = actT[0].rearrange("p a b -> p (a b)")
            for tt in range(4):
                dma(xnew_d[tok0 + tt * 128: tok0 + (tt + 1) * 128, :], xt[:, tt, :], reads=[Bxt[tt]], eng="pool", sem_buf=Bxt[tt])
                act(junk, xt[:, tt, :], AF.Square, reads=[Bxt[tt]], writes=[Bss[tt], BaT[0]], accum_out=ssq[:, tt:tt + 1])
                act(rstd[:, tt:tt + 1], ssq[:, tt:tt + 1], AF.Sqrt, reads=[Bss[tt], B_c2], writes=[Brs[tt]], scale=1.0 / D, bias=epsb[:, 0:1])
                vop("dve", "reciprocal", reads=[Brs[tt]], writes=[Brs[tt]], out=rstd[:, tt:tt + 1], in_=rstd[:, tt:tt + 1])
                vop("pool", "tensor_scalar", reads=[Brs[tt]], writes=[Bxt[tt]], out=xt[:, tt, :], in0=xt[:, tt, :],
                    scalar1=rstd[:, tt:tt + 1], scalar2=None, op0=ALU.mult)
            for k in range(16):
                bank = 2 + k % 2
                for tt in range(4):
                    tr(ps[bank][:, tt * 128:(tt + 1) * 128], xt[:, tt, k * 128:(k + 1) * 128], identf, reads=[Bxt[tt], B_const],
                       partial=[PB[bank]])
                act(h2T32[:, k, :], ps[bank][:, :], AF.Identity, reads=[PB[bank], B_cols], partial=[BR[2], BR[3]],
                    scale=cols[:, 4, k:k + 1], bias=cols[:, 5, k:k + 1])
                if k % 4 == 3:
                    vop("pool", "tensor_copy", reads=[BR[2], BR[3]], partial=[Bh2], out=h2T[:, k - 3:k + 1, :], in_=h2T32[:, k - 3:k + 1, :])
            for tt in range(4):
                for k in range(16):
                    mm(ps[4][:, 0:NE], h2T32[:, k, tt * 128:(tt + 1) * 128], wr32[:, k, :], k == 0, k == 15,
                       reads=[BR[2], BR[3], Bc5], partial=[PB[4]])
                act(sg, ps[4][:, 0:NE], AF.Sigmoid, reads=[PB[4]], writes=[Bsm])
                dv = lambda name, **kw: vop("dve", name, reads=[Bsm, Bc5], writes=[Bsm], **kw)
                dv("tensor_tensor", out=sel, in0=sg, in1=rbias, op=ALU.add)
                for g in range(8):
                    dv("max", out=gs8[:, g, :], in_=sel[:, g * 8:(g + 1) * 8])
                dv("tensor_tensor", out=gsc, in0=gs8[:, :, 0], in1=gs8[:, :, 1], op=ALU.add)
                dv("max", out=top8, in_=gsc)
                dv("tensor_scalar", out=gm, in0=gsc, scalar1=top8[:, 3:4], scalar2=None, op0=ALU.is_ge)
                dv("tensor_scalar", out=pen, in0=gm, scalar1=-1.0, scalar2=1e9, op0=ALU.add, op1=ALU.mult)
                for g in range(8):
                    dv("tensor_scalar", out=selm[:, g * 8:(g + 1) * 8], in0=sel[:, g * 8:(g + 1) * 8], scalar1=gm[:, g:g + 1],
                       scalar2=pen[:, g:g + 1], op0=ALU.mult, op1=ALU.add)
                dv("max", out=top8b, in_=selm)
                dv("tensor_scalar", out=ch, in0=selm, scalar1=top8b[:, 7:8], scalar2=None, op0=ALU.is_ge)
                dv("tensor_tensor", out=wv, in0=sg, in1=ch, op=ALU.mult)
                dv("tensor_reduce", out=wsum, in_=wv, axis=AX.X, op=ALU.add)
                dv("reciprocal", out=rws, in_=wsum)
                vop("dve", "tensor_scalar", reads=[Bsm], partial=[BG[tt]], out=G[:, tt, 0:NE], in0=wv, scalar1=rws, scalar2=2.5,
                    op0=ALU.mult, op1=ALU.mult)
            if dbg and s == 0:
                dma(dbg_out("dbg_G", [128, 4 * (NE + 1)]), G.rearrange("p a b -> p (a b)"), reads=BG)
            vop("pool", "memset", writes=allY, ap=yacc.rearrange("p a b -> p (a b)"), constant=0.0)
            dcnt = 0
            for e in range(NEXP):
                sg_ = load_unit(wexp_d[0][e], 128, 8192)
                su_ = load_unit(wexp_d[1][e], 128, 8192)
                sd_ = load_unit(wexp_d[2][e], 128, 8192)
                Wg = ring[sg_].rearrange("p (k f) -> p k f", k=16)
                Wu = ring[su_].rearrange("p (k f) -> p k f", k=16)
                Wd = ring[sd_].rearrange("p (k n) -> p k n", k=4)
                ai = e % 2
                for f in range(4):
                    gb = f % 2
                    ub = 2 + f % 2
                    for k in range(16):
                        mm(ps[gb][:, :], Wg[:, k, f * 128:(f + 1) * 128], h2T[:, k, :], k == 0, k == 15,
                           reads=[BR[sg_], Bh2], partial=[PB[gb]])
                    for k in range(16):
                        mm(ps[ub][:, :], Wu[:, k, f * 128:(f + 1) * 128], h2T[:, k, :], k == 0, k == 15,
                           reads=[BR[su_], Bh2], partial=[PB[ub]])
                    si = f % 2
                    act(stmp[si], ps[gb][:, :], AF.Silu, reads=[PB[gb]], writes=[Bst[si]])
                    vop("dve", "tensor_tensor", reads=[Bst[si], PB[ub]], partial=[BaT[ai]], out=actT[ai][:, f, :], in0=stmp[si],
                        in1=ps[ub][:, :], op=ALU.mult)
                ecol = e if e < NE else NE
                for tt in range(4):
                    for n in range(4):
                        bank = 4 + dcnt % 3
                        dcnt += 1
                        for f in range(4):
                            mm(ps[bank][:, :], actT[ai][:, f, tt * 128:(tt + 1) * 128], Wd[:, f, n * 512:(n + 1) * 512], f == 0, f == 3,
                               reads=[BaT[ai], BR[sd_]], partial=[PB[bank]])
                        ys = yacc[:, tt, n * 512:(n + 1) * 512]
                        vop("dve", "scalar_tensor_tensor", reads=[PB[bank], BG[tt]], writes=[By[tt][n]], out=ys, in0=ps[bank][:, :],
                            scalar=G[:, tt, ecol:ecol + 1], in1=ys, op0=ALU.mult, op1=ALU.add)
            for tt in range(4):
                dma(xt[:, tt, :], xnew_d[tok0 + tt * 128: tok0 + (tt + 1) * 128, :], writes=[Bxt[tt]])
                vop("dve", "tensor_tensor", reads=By[tt] + [B_gf], writes=By[tt], out=yacc[:, tt, :], in0=yacc[:, tt, :], in1=gf_bc, op=ALU.mult)
                vop("pool", "tensor_tensor", reads=By[tt], writes=[Bxt[tt]], out=xt[:, tt, :], in0=xt[:, tt, :], in1=yacc[:, tt, :], op=ALU.add)
                j = 4 + tt
                act(junk, xt[:, tt, :], AF.Square, reads=[Bxt[tt]], writes=[Bss[j], BaT[0]], accum_out=ssq[:, j:j + 1])
                act(rstd[:, j:j + 1], ssq[:, j:j + 1], AF.Sqrt, reads=[Bss[j], B_c2], writes=[Brs[j]], scale=1.0 / D, bias=epsb[:, 0:1])
                vop("dve", "reciprocal", reads=[Brs[j]], writes=[Brs[j]], out=rstd[:, j:j + 1], in_=rstd[:, j:j + 1])
                vop("dve", "scalar_tensor_tensor", reads=[Bxt[tt], Brs[j], Bc5], writes=By[tt], out=yacc[:, tt, :], in0=xt[:, tt, :],
                    scalar=rstd[:, j:j + 1], in1=gfin, op0=ALU.mult, op1=ALU.mult)
                dma(out_d[tok0 + tt * 128: tok0 + (tt + 1) * 128, :], yacc[:, tt, :], reads=By[tt], eng="pool", sem_buf=By[tt][0])
        S.barrier()
        cv.release(m0)

    NEXP = NE + 1
    if stage >= 4:
        phase5()

    S.barrier()
    S.resolve()
    sems = {s: es.enter_context(nc.semaphore("sem_" + s)) for s in S.streams}
    with nc.Block() as block:
        S.emit(block, sems)
    es.close()
    info = {"peak_sbuf": cv.peak, "n_ops": {e: len(S.ops[e]) for e in ENGS}, "n_sems": len(sems)}
    return nc, dbg_outs, info


def rope_tables(half):
    j = np.arange(SEQ)
    t = (j + half * OWN) % SEQ
    r = (t // 64).astype(np.float32)
    cidx = (t % 64).astype(np.float32)
    inv = (10000.0 ** (-np.arange(16, dtype=np.float32) / 16)).astype(np.float32)
    ang = np.concatenate([r[:, None] * inv, cidx[:, None] * inv], axis=-1).astype(np.float32)
    cos = np.cos(ang).astype(np.float32).T
    sin = np.sin(ang).astype(np.float32).T
    C = np.concatenate([cos, cos], axis=0)
    Sg = np.concatenate([-sin, sin], axis=0)
    return np.ascontiguousarray(C), np.ascontiguousarray(Sg)


def window_masks(half):
    m = np.zeros((8, 128, 512), np.float32)
    r = np.arange(128)[:, None]
    c = np.arange(512)[None, :]
    for jj in range(6):
        kp = (jj - 1) * 128 + r
        m[jj] = (np.abs(kp - c) <= 128)
    m[6] = m[0] if half == 1 else 0.0
    m[7] = m[5] if half == 0 else 0.0
    return m.astype(ml_dtypes.bfloat16)


def make_in_maps(inp, nei=NE):
    f = lambda a: np.ascontiguousarray(np.asarray(a, dtype=np.float32))
    x = f(inp["x"]); c = f(inp["c"]); ctx = f(inp["ctx"]); c_ctx = f(inp["c_ctx"])
    shared = {
        "w_mod": f(inp["w_mod"][0]),
        "b_mod2": f(np.stack([inp["b_mod"][0], inp["b_mod"][0]])),
        "gcol": f(np.concatenate([np.asarray(inp["norm_attn_g"][0]).reshape(16, 128).T,
                                  np.asarray(inp["norm_ffn_g"][0]).reshape(16, 128).T], axis=1)),
        "w_in": f(inp["w_in"][0]),
        "sink": f(np.broadcast_to(np.asarray(inp["attn_sink"][0])[None, :], (128, 16))),
        "gq": f(np.broadcast_to(np.asarray(inp["q_a_norm_g"][0])[None, :], (128, 512))),
        "gkv": f(np.broadcast_to(np.asarray(inp["kv_a_norm_g"][0])[None, :], (128, 256))),
        "w_uq": f(inp["w_uq"][0]), "w_ukv": f(inp["w_ukv"][0]), "w_out": f(inp["w_out"][0]),
        "w_router": f(inp["w_router"][0]),
        "rbias": f(np.broadcast_to(np.asarray(inp["router_bias"][0])[None, :], (128, NE))),
        "w_gate": f(inp["w_gate"][0][:nei]), "w_up": f(inp["w_up"][0][:nei]), "w_down": f(inp["w_down"][0][:nei]),
        "ws_gate": f(inp["ws_gate"][0]), "ws_up": f(inp["ws_up"][0]), "ws_down": f(inp["ws_down"][0]),
        "gfin": f(np.broadcast_to(np.asarray(inp["norm_final_g"])[None, :], (128, D))),
        "identb": np.eye(128, dtype=np.float32).astype(ml_dtypes.bfloat16),
        "identf": np.eye(128, dtype=np.float32),
        "perm64": np.ascontiguousarray(np.roll(np.eye(64, dtype=np.float32), 32, axis=0)),
    }
    maps = []
    for core in range(8):
        b, half = core // 2, core % 2
        own = slice(half * OWN, (half + 1) * OWN)
        oth = slice((1 - half) * OWN, (2 - half) * OWN)
        C, Sg = rope_tables(half)
        cT = np.stack([c[b].reshape(16, 128).T, c_ctx.reshape(16, 128).T], axis=-1).reshape(128, 32)
        m = dict(shared)
        m.update({
            "xs": np.ascontiguousarray(np.concatenate([x[b, own], x[b, oth]], axis=0)),
            "ctxb": np.ascontiguousarray(ctx[b]),
            "cT": np.ascontiguousarray(cT),
            "ropeC": C, "ropeS": Sg,
            "masks": window_masks(half),
        })
        maps.append(m)
    return maps


_CACHE = {}


def kernel(**inputs):
    if "nc" not in _CACHE:
        _CACHE["nc"] = build()[0]
    nc = _CACHE["nc"]
    maps = make_in_maps(inputs)
    res = run_bass_kernel_spmd(nc, maps, core_ids=list(range(8)))
    out = np.empty((4, SEQ, D), np.float32)
    for core in range(8):
        b, half = core // 2, core % 2
        out[b, half * OWN:(half + 1) * OWN] = res.results[core]["out"]
    return out
```
